# Optimizing a Trainium2 kernel written in Bass

```python
import jax, jax.numpy as jnp
from jax import lax
import numpy as np

D_MODEL = 2048
BATCH = 1
SEQ = 8192
DEPTH = 1
DEC_BATCH = 8
DEC_SEQ = 4096
PAST_LEN = 128

MIX_WIDTH = D_MODEL
RET_WIDTH = MIX_WIDTH // 2
GLA_WIDTH = MIX_WIDTH - RET_WIDTH
RET_HEADS = 4
RET_DK = RET_WIDTH // RET_HEADS
RET_DV = RET_WIDTH // RET_HEADS
GLA_HEADS = 4
GLA_DK = GLA_WIDTH // (2 * GLA_HEADS)
GLA_DV = GLA_WIDTH // GLA_HEADS
GLA_KEY_WIDTH = GLA_HEADS * GLA_DK
GLA_RANK = 16
GLA_TAU = 16.0
RET_CHUNK = 128
GLA_CHUNK = 64
ROPE_BASE = 10000.0
N_EXPERTS = 16
CAPACITY_FACTOR = 2
D_FF_EXPERT = D_MODEL
EPS = 1e-6
IN_WIDTH = 4 * RET_WIDTH + 2 * GLA_KEY_WIDTH + 2 * GLA_WIDTH + 2 * GLA_RANK

kernel_name = 'hybrid_retention_gla_ec_moe_encoder'


def rmsnorm(x, w):
    xf = x.astype(jnp.float32)
    y = xf * lax.rsqrt(jnp.mean(xf * xf, axis=-1, keepdims=True) + EPS)
    return (y * w.astype(jnp.float32)).astype(x.dtype)


def head_rmsnorm(y, w):
    H, dv = y.shape[-2], y.shape[-1]
    yn = y * lax.rsqrt(jnp.mean(y * y, axis=-1, keepdims=True) + EPS)
    return yn * w.astype(jnp.float32).reshape(H, dv)


def rope(x):
    L, d = x.shape[1], x.shape[-1]
    inv = ROPE_BASE ** (-jnp.arange(0, d, 2, dtype=jnp.float32) / d)
    ang = jnp.arange(L, dtype=jnp.float32)[:, None] * inv[None, :]
    cos = jnp.cos(ang)[None, :, None, :]
    sin = jnp.sin(ang)[None, :, None, :]
    x1, x2 = x[..., : d // 2], x[..., d // 2:]
    return jnp.concatenate([x1 * cos - x2 * sin, x1 * sin + x2 * cos], axis=-1)


def _chunks(t, c):
    B, L, H, d = t.shape
    return t.reshape(B, L // c, c, H, d).transpose(1, 0, 3, 2, 4)


def _unchunk(t):
    N, B, H, C, d = t.shape
    return t.transpose(1, 0, 3, 2, 4).reshape(B, N * C, H, d)


def retention_scan(q, k, v, log_gamma):
    B, L, H, dk = q.shape
    dv = v.shape[-1]
    C = RET_CHUNK
    pos = jnp.arange(C, dtype=jnp.float32)
    diff = pos[:, None] - pos[None, :]
    lg = log_gamma[:, None, None]
    intra = jnp.exp(jnp.where(diff >= 0, lg * diff, -jnp.inf))
    q_dec = jnp.exp(log_gamma[:, None] * (pos + 1.0))[..., None]
    k_dec = jnp.exp(log_gamma[:, None] * (C - 1.0 - pos))[..., None]
    c_dec = jnp.exp(log_gamma * C)[:, None, None]

    def step(S, inp):
        qi, ki, vi = inp
        scores = jnp.einsum('bhid,bhjd->bhij', qi, ki) * intra
        o = (jnp.einsum('bhij,bhjv->bhiv', scores, vi)
             + jnp.einsum('bhid,bhdv->bhiv', qi, S) * q_dec)
        S = S * c_dec + jnp.einsum('bhjd,bhjv->bhdv', ki * k_dec, vi)
        return S, o

    S0 = jnp.zeros((B, H, dk, dv), jnp.float32)
    _, o = lax.scan(step, S0, (_chunks(q, C), _chunks(k, C), _chunks(v, C)))
    return _unchunk(o)


def gla_scan(q, k, v, log_a):
    B, L, H, dk = q.shape
    dv = v.shape[-1]
    C = GLA_CHUNK
    causal = jnp.tril(jnp.ones((C, C), dtype=bool))[:, :, None]

    def step(S, inp):
        qi, ki, vi, ai = inp
        b = jnp.cumsum(ai, axis=2)
        expo = b[:, :, :, None, :] - b[:, :, None, :, :]
        w = jnp.exp(jnp.where(causal, expo, -jnp.inf))
        scores = jnp.einsum('bhid,bhijd->bhij', qi, ki[:, :, None, :, :] * w)
        b_last = b[:, :, -1:, :]
        o = (jnp.einsum('bhij,bhjv->bhiv', scores, vi)
             + jnp.einsum('bhid,bhdv->bhiv', qi * jnp.exp(b), S))
        S = (jnp.exp(b_last[:, :, 0, :])[..., None] * S
             + jnp.einsum('bhjd,bhjv->bhdv', ki * jnp.exp(b_last - b), vi))
        return S, o

    S0 = jnp.zeros((B, H, dk, dv), jnp.float32)
    _, o = lax.scan(step, S0, (_chunks(q, C), _chunks(k, C), _chunks(v, C), _chunks(log_a, C)))
    return _unchunk(o)


def _flip(t):
    return jnp.flip(t, axis=1)


def token_mixers(xn, w_in, ret_decay_logit, ret_gn_w, gla_gate_up, gla_gate_bias, gla_gn_w, w_out):
    B, L, _ = xn.shape
    f32 = jnp.float32
    proj = jnp.einsum('bld,dn->bln', xn, w_in).astype(f32)
    sizes = [RET_WIDTH] * 4 + [GLA_KEY_WIDTH, GLA_KEY_WIDTH, GLA_WIDTH, GLA_WIDTH, 2 * GLA_RANK]
    points = [int(p) for p in np.cumsum(sizes)[:-1]]
    rq, rk, rv, rg, gq, gk, gv, gg, ga = jnp.split(proj, points, axis=-1)

    rq = rope(rq.reshape(B, L, RET_HEADS, RET_DK)) * (RET_DK ** -0.5)
    rk = rope(rk.reshape(B, L, RET_HEADS, RET_DK))
    rv = rv.reshape(B, L, RET_HEADS, RET_DV)
    log_gamma = jax.nn.log_sigmoid(ret_decay_logit.astype(f32))
    ret = (retention_scan(rq, rk, rv, log_gamma[0])
           + _flip(retention_scan(_flip(rq), _flip(rk), _flip(rv), log_gamma[1])))
    ret_out = head_rmsnorm(ret, ret_gn_w).reshape(B, L, RET_WIDTH) * jax.nn.silu(rg)

    gq = gq.reshape(B, L, GLA_HEADS, GLA_DK) * (GLA_DK ** -0.5)
    gk = gk.reshape(B, L, GLA_HEADS, GLA_DK)
    gv = gv.reshape(B, L, GLA_HEADS, GLA_DV)
    up = gla_gate_up.astype(f32)
    bias = gla_gate_bias.astype(f32)
    la_f = jax.nn.log_sigmoid(jnp.einsum('blr,rk->blk', ga[..., :GLA_RANK], up[0]) + bias[0]) / GLA_TAU
    la_b = jax.nn.log_sigmoid(jnp.einsum('blr,rk->blk', ga[..., GLA_RANK:], up[1]) + bias[1]) / GLA_TAU
    la_f = la_f.reshape(B, L, GLA_HEADS, GLA_DK)
    la_b = la_b.reshape(B, L, GLA_HEADS, GLA_DK)
    gla = (gla_scan(gq, gk, gv, la_f)
           + _flip(gla_scan(_flip(gq), _flip(gk), _flip(gv), _flip(la_b))))
    gla_out = head_rmsnorm(gla, gla_gn_w).reshape(B, L, GLA_WIDTH) * jax.nn.silu(gg)

    mix = jnp.concatenate([ret_out, gla_out], axis=-1).astype(xn.dtype)
    return jnp.einsum('bln,nd->bld', mix, w_out)


def expert_choice_ffn(xn, router_w, w_gate, w_up, w_down):
    B, L, D = xn.shape
    T = B * L
    cap = CAPACITY_FACTOR * T // N_EXPERTS
    xt = xn.reshape(T, D)
    logits = jnp.einsum('td,de->te', xt, router_w).astype(jnp.float32)
    affinity = jax.nn.softmax(logits, axis=-1)
    gate, idx = lax.top_k(affinity.T, cap)
    xe = xt[idx]
    hdn = (jax.nn.silu(jnp.einsum('ecd,edf->ecf', xe, w_gate))
           * jnp.einsum('ecd,edf->ecf', xe, w_up))
    ye = jnp.einsum('ecf,efd->ecd', hdn, w_down) * gate[..., None].astype(xn.dtype)
    out = jnp.zeros((T, D), ye.dtype).at[idx.reshape(-1)].add(ye.reshape(-1, D))
    return out.reshape(B, L, D).astype(xn.dtype)


def encoder_trunk(x, norm1_w, w_in, ret_decay_logit, ret_gn_w, gla_gate_up, gla_gate_bias,
                  gla_gn_w, w_out, norm2_w, router_w, w_gate, w_up, w_down, normf_w):
    for layer in range(DEPTH):
        x = x + token_mixers(rmsnorm(x, norm1_w[layer]), w_in[layer], ret_decay_logit[layer],
                             ret_gn_w[layer], gla_gate_up[layer], gla_gate_bias[layer],
                             gla_gn_w[layer], w_out[layer])
        x = x + expert_choice_ffn(rmsnorm(x, norm2_w[layer]), router_w[layer],
                                  w_gate[layer], w_up[layer], w_down[layer])
    return rmsnorm(x, normf_w)


def setup_inputs(seed: int = 0) -> dict:
    key = jax.random.key(seed)
    ks = jax.random.split(key, 18)
    f32 = jnp.float32
    nrm = lambda k, shape: jax.random.normal(k, shape, f32)
    base_logit = jnp.log(2.0 ** (5.0 + jnp.arange(RET_HEADS, dtype=f32)) - 1.0)
    return {
        'x_prompt': nrm(ks[0], (BATCH, SEQ, D_MODEL)),
        'x_sample': nrm(ks[1], (DEC_BATCH, DEC_SEQ, D_MODEL)),
        'norm1_w': 1.0 + 0.02 * nrm(ks[2], (DEPTH, D_MODEL)),
        'w_in': nrm(ks[3], (DEPTH, D_MODEL, IN_WIDTH)) * D_MODEL ** -0.5,
        'ret_decay_logit': base_logit + 0.1 * nrm(ks[4], (DEPTH, 2, RET_HEADS)),
        'ret_gn_w': 1.0 + 0.02 * nrm(ks[5], (DEPTH, RET_WIDTH)),
        'gla_gate_up': nrm(ks[6], (DEPTH, 2, GLA_RANK, GLA_KEY_WIDTH)) * GLA_RANK ** -0.5,
        'gla_gate_bias': 0.1 * nrm(ks[7], (DEPTH, 2, GLA_KEY_WIDTH)),
        'gla_gn_w': 1.0 + 0.02 * nrm(ks[8], (DEPTH, GLA_WIDTH)),
        'w_out': nrm(ks[9], (DEPTH, MIX_WIDTH, D_MODEL)) * MIX_WIDTH ** -0.5,
        'norm2_w': 1.0 + 0.02 * nrm(ks[10], (DEPTH, D_MODEL)),
        'router_w': nrm(ks[11], (DEPTH, D_MODEL, N_EXPERTS)) * D_MODEL ** -0.5,
        'w_gate': nrm(ks[12], (DEPTH, N_EXPERTS, D_MODEL, D_FF_EXPERT)) * D_MODEL ** -0.5,
        'w_up': nrm(ks[13], (DEPTH, N_EXPERTS, D_MODEL, D_FF_EXPERT)) * D_MODEL ** -0.5,
        'w_down': nrm(ks[14], (DEPTH, N_EXPERTS, D_FF_EXPERT, D_MODEL)) * D_FF_EXPERT ** -0.5,
        'normf_w': 1.0 + 0.02 * nrm(ks[15], (D_MODEL,)),
    }


def reference(x_prompt, x_sample, norm1_w, w_in, ret_decay_logit, ret_gn_w, gla_gate_up,
              gla_gate_bias, gla_gn_w, w_out, norm2_w, router_w, w_gate, w_up, w_down, normf_w):
    y_prompt = encoder_trunk(x_prompt, norm1_w, w_in, ret_decay_logit, ret_gn_w, gla_gate_up,
                             gla_gate_bias, gla_gn_w, w_out, norm2_w, router_w, w_gate, w_up,
                             w_down, normf_w)
    y_sample = encoder_trunk(x_sample, norm1_w, w_in, ret_decay_logit, ret_gn_w, gla_gate_up,
                             gla_gate_bias, gla_gn_w, w_out, norm2_w, router_w, w_gate, w_up,
                             w_down, normf_w)
    return (y_prompt, y_sample)
```

```python
import contextlib
import numpy as np
import ml_dtypes
import concourse.bass as bass
import concourse.mybir as mybir
from concourse.bass_utils import run_bass_kernel_spmd

F32 = mybir.dt.float32
BF16 = mybir.dt.bfloat16
I32 = mybir.dt.int32
U32 = mybir.dt.uint32
ALU = mybir.AluOpType
AF = mybir.ActivationFunctionType
AX = mybir.AxisListType

D = 2048
KC = 16
INW = 7200
NE = 16
C = 128
EPS = 1e-6
NCORES = 8

O_RQ, O_RK, O_RV, O_RG, O_GQ, O_GK, O_GV, O_GG, O_GA = 0, 1024, 2048, 3072, 4096, 4608, 5120, 6144, 7168
FM_COLS = ([O_RQ + 128 * b for b in range(8)] + [O_RK + 128 * b for b in range(8)]
           + [O_GQ + 128 * b for b in range(4)] + [O_GK + 128 * b for b in range(4)])
TM_COLS = [O_RV, O_RV + 512, O_RG, O_RG + 512, O_GV, O_GV + 512, O_GG, O_GG + 512]
TM_GATE = [False, False, True, True, False, False, True, True]
HEADS = []
for h in range(4):
    HEADS.append(dict(nd=2, qb=[2 * h, 2 * h + 1], kb=[8 + 2 * h, 9 + 2 * h], v=256 * h, g=1024 + 256 * h, ret=True, h=h))
for h in range(4):
    HEADS.append(dict(nd=1, qb=[16 + h], kb=[20 + h], v=2048 + 256 * h, g=3072 + 256 * h, ret=False, h=h))


class Sched:
    LIMIT = 30000

    def __init__(self, nc, stack):
        self.nc = nc
        self.stack = stack
        self.sem = {}
        self.cnt = {}
        self.gen = {}
        self.last = None

    def _key(self, key):
        if key not in self.sem or self.cnt[key] > self.LIMIT:
            g = self.gen.get(key, 0)
            self.gen[key] = g + 1
            self.sem[key] = self.stack.enter_context(self.nc.semaphore("s_%s_%d_%d" % (key[0], int(key[1]), g)))
            self.cnt[key] = 0

    def op(self, eng, fn, dma=False, wait=True, inc=True, incv=None):
        e = getattr(self.nc, eng)
        key = (eng, dma)
        if wait and self.last is not None:
            e.wait_ge(self.last[0], self.last[1])
        ins = fn(e)
        if inc:
            self._key(key)
            v = incv if incv is not None else (16 if dma else 1)
            self.cnt[key] += v
            ins.then_inc(self.sem[key], v)
            self.last = (self.sem[key], self.cnt[key])
        return ins

    def dma(self, out, in_, eng="sync", **kw):
        return self.op(eng, lambda e: e.dma_start(out=out, in_=in_, **kw), dma=True)

    def mm(self, out, lhsT, rhs, start, stop, first, last):
        return self.op("tensor", lambda e: e.matmul(out, lhsT, rhs, start=start, stop=stop),
                       wait=first, inc=last)

    def tr(self, out, in_, ident, first=True, last=True):
        return self.op("tensor", lambda e: e.transpose(out, in_, ident), wait=first, inc=last)

    def v(self, fn):
        return self.op("vector", fn)

    def a(self, fn):
        return self.op("scalar", fn)

    def g(self, fn):
        return self.op("gpsimd", fn)


def build(Ls, Lp, phase, debug=False):
    NT = Ls + Lp
    NCH = NT // C
    TS = NCORES * Ls
    capS = 2 * TS // NE
    capP = 2 * Lp // NE
    JS = TS // 128
    JP = Lp // 128
    LQ = Lp // NCORES
    nc = bass.Bass("TRN2", target_bir_lowering=False, num_devices=NCORES)
    dt = nc.dram_tensor

    def ein(name, shape, dtype):
        return dt(name, shape, dtype, kind="ExternalInput").ap()

    def eout(name, shape, dtype):
        return dt(name, shape, dtype, kind="ExternalOutput").ap()

    if phase == 1:
        x_in = ein("x_in", [NT, D], F32)
        w_in = ein("w_in", [D, INW], F32)
        w_out = ein("w_out", [D, D], F32)
        n1w = ein("n1w", [128, KC], F32)
        gnw = ein("gnw", [128, KC], F32)
        declog = ein("declog", [1, 8], F32)
        upb = ein("upb", [33, 1024], F32)
        rw = ein("rw", [128, KC * NE], F32)
        ropec = ein("ropec", [128, NT], F32)
        ropes = ein("ropes", [128, NT], F32)
        hbuf = eout("hbuf", [NT, D], F32)
        xn_l = eout("xn_l", [Ls, D], BF16)
        xn_p = eout("xn_p", [Lp, D], BF16)
        affb = eout("affb", [NE, Ls], F32)
        affx_p = eout("affx_p", [NE * 128, JP], F32)
        winb = dt("winb", [D, INW], BF16).ap()
        qkt = dt("qkt", [24, 128, NT], BF16).ap()
        tms = dt("tms", [NT, 4096], BF16).ap()
        gat = dt("gat", [32, NT], F32).ap()
        ofw = dt("ofw", [NT, D], F32).ap()
    if phase == 2:
        wg = ein("wg", [2, D, D], F32)
        wu = ein("wu", [2, D, D], F32)
        wd = ein("wd", [2, D, D], F32)
        eidx = ein("eidx", [128, 2], I32)
        aux = ein("aux", [128, 258], F32)
        xn_s = ein("xn_s", [TS, D], BF16)
        xn_p = ein("xn_p", [Lp, D], BF16)
        affx_s = ein("affx_s", [NE * 128, JS], F32)
        affx_p = ein("affx_p", [NE * 128, JP], F32)
        out_s = eout("out_s", [TS, D], F32)
        out_p = eout("out_p", [Lp, D], F32)
        wgb = dt("wgb", [2, D, D], BF16).ap()
        wub = dt("wub", [2, D, D], BF16).ap()
        wdb = dt("wdb", [2, D, D], BF16).ap()
    if phase in (1, 2):
        n2w = ein("n2w", [128, KC], F32)
        ctab = ein("ctab", [128, 8 * 128], F32)
        identf = ein("identf", [128, 128], F32)
    if phase == 3:
        NR = Ls + LQ
        nfw = ein("nfw", [1, D], F32)
        h_own = ein("h_own", [NR, D], F32)
        contrib = ein("contrib", [NCORES, NR, D], F32)
        y_o = eout("y_o", [NR, D], F32)

    st = contextlib.ExitStack()
    with st:
        S = Sched(nc, st)

        def sb(name, shape, dtype):
            return st.enter_context(nc.sbuf_tensor(name, shape, dtype))

        def ps(name, shape, dtype):
            return st.enter_context(nc.psum_tensor(name, shape, dtype))

        if phase in (1, 2):
            ctab_s = sb("ctab_s", [128, 8 * 128], F32)
            identf_s = sb("identf_s", [128, 128], F32)
            identb_s = sb("identb_s", [128, 128], BF16)
            n2w_s = sb("n2w_s", [128, KC], F32)
            S.dma(ctab_s[:], ctab)
            S.dma(identf_s[:], identf)
            S.dma(n2w_s[:], n2w)
            S.v(lambda e: e.tensor_copy(identb_s[:], identf_s[:]))
            POSF, POSB, CF, CB, MF, MB, UF, UB = [ctab_s[:, 128 * i:128 * (i + 1)] for i in range(8)]
            psA = ps("psA", [128, 512], F32)
            psB = ps("psB", [128, 512], F32)
            psT = ps("psT", [128, 1024], BF16)
            psTf = ps("psTf", [128, 512], F32)
            psS = ps("psS", [128, 128], F32)
            psO = ps("psO", [128, 256], F32)
            psSt = ps("psSt", [128, 2, 256], F32)
            psZ = ps("psZ", [128, 512], F32)
        RB = 128
        if phase == 1:
            n1w_s = sb("n1w_s", [128, KC], F32)
            gnw_s = sb("gnw_s", [128, KC], F32)
            lg_s = sb("lg_s", [128, 8], F32)
            upb_s = sb("upb_s", [33, 1024], F32)
            rw_s = sb("rw_s", [128, KC * NE], F32)
            S.dma(n1w_s[:], n1w)
            S.dma(gnw_s[:], gnw)
            S.dma(upb_s[:], upb)
            S.dma(rw_s[:], rw)
            S.dma(lg_s[:], declog.to_broadcast([128, 8]))
            S.a(lambda e: e.activation(lg_s[:], lg_s[:], AF.Exp, scale=-1.0))
            S.a(lambda e: e.activation(lg_s[:], lg_s[:], AF.Ln, bias=1.0))
            S.v(lambda e: e.tensor_scalar(lg_s[:], lg_s[:], -1.0, None, ALU.mult))
            rtab = sb("rtab", [128, 8, 3, 128], F32)
            rlast = sb("rlast", [128, 8], F32)
            for dr in range(2):
                pos = POSF if dr == 0 else POSB
                cpl = CF if dr == 0 else CB
                for h in range(4):
                    i = dr * 4 + h
                    sc = lg_s[:, i:i + 1]
                    S.a(lambda e: e.activation(rtab[:, i, 0, :], pos, AF.Exp, scale=sc))
                    S.v(lambda e: e.reciprocal(rtab[:, i, 1, :], rtab[:, i, 0, :]))
                    S.v(lambda e: e.tensor_scalar(rtab[:, i, 0, :], rtab[:, i, 0, :], 1.0 / 16.0, None, ALU.mult))
                    S.a(lambda e: e.activation(rtab[:, i, 2, :], cpl, AF.Exp, scale=sc))
            S.a(lambda e: e.activation(rlast[:], lg_s[:], AF.Exp, scale=float(C)))

        if phase == 1:
            for r0 in range(0, D, RB):
                S.dma(winb[r0:r0 + RB, :].rearrange("r (a b) -> r a b", b=1800),
                      w_in[r0:r0 + RB, :].rearrange("r (a b) -> r a b", b=1800), eng="gpsimd")

            TT = 512 if NT % 512 == 0 else 256
            with contextlib.ExitStack() as p1:
                def sb1(name, shape, dtype):
                    return p1.enter_context(nc.sbuf_tensor(name, shape, dtype))
                wgrp = sb1("wgrp", [128, KC, 512], BF16)
                xnT = sb1("xnT", [128, KC, TT], BF16)
                xt = sb1("xt", [128, D], F32)
                xnb = sb1("xnb", [128, D], BF16)
                sq = sb1("sq", [128, D], F32)
                ssum = sb1("ssum", [128, 1], F32)
                cs_c = sb1("cs_c", [128, TT], F32)
                cs_s = sb1("cs_s", [128, TT], F32)
                fm1 = sb1("fm1", [128, TT], BF16)
                fm2 = sb1("fm2", [128, TT], BF16)
                t1 = sb1("t1", [128, TT], F32)
                t2 = sb1("t2", [128, TT], F32)
                tmo = sb1("tmo", [128, 512], BF16)
                gao = sb1("gao", [32, TT], F32)
                winv = winb.rearrange("(kc p) n -> p kc n", p=128)
                for t0 in range(0, NT, TT):
                    for c0 in range(0, TT, C):
                        S.dma(xt[:], x_in[t0 + c0:t0 + c0 + C, :])
                        S.v(lambda e: e.tensor_tensor(sq[:], xt[:], xt[:], ALU.mult))
                        S.v(lambda e: e.reduce_sum(ssum[:], sq[:], AX.X))
                        S.v(lambda e: e.tensor_scalar(ssum[:], ssum[:], 1.0 / D, EPS, ALU.mult, ALU.add))
                        S.a(lambda e: e.activation(ssum[:], ssum[:], AF.Sqrt))
                        S.v(lambda e: e.reciprocal(ssum[:], ssum[:]))
                        S.v(lambda e: e.tensor_scalar(xnb[:], xt[:], ssum[:, 0:1], None, ALU.mult))
                        for k0 in range(0, KC, 8):
                            for kk in range(8):
                                kc = k0 + kk
                                S.tr(psT[:, kk * 128:(kk + 1) * 128], xnb[:, kc * 128:(kc + 1) * 128], identb_s[:],
                                     first=(kk == 0), last=(kk == 7))
                            for kk in range(8):
                                kc = k0 + kk
                                S.v(lambda e: e.tensor_scalar(xnT[:, kc, c0:c0 + C], psT[:, kk * 128:(kk + 1) * 128],
                                                              n1w_s[:, kc:kc + 1], None, ALU.mult))
                    S.dma(cs_c[:], ropec[:, t0:t0 + TT])
                    S.dma(cs_s[:], ropes[:, t0:t0 + TT])
                    for g0 in (O_RQ, O_RQ + 512, O_RK, O_RK + 512, O_GQ, O_GK):
                        S.dma(wgrp[:], winv[:, :, g0:g0 + 512])
                        blks = [b for b in range(24) if g0 <= FM_COLS[b] < g0 + 512]
                        for pi in range(0, 4, 2):
                            pair = blks[pi:pi + 2]
                            for j, b in enumerate(pair):
                                co = FM_COLS[b] - g0
                                pt = psA if j == 0 else psB
                                for kc in range(KC):
                                    S.mm(pt[:, 0:TT], wgrp[:, kc, co:co + 128], xnT[:, kc, :], kc == 0, kc == KC - 1,
                                         first=(kc == 0), last=(kc == KC - 1))
                            if g0 < O_GQ:
                                S.v(lambda e: e.tensor_tensor(t1[:], psA[:, 0:TT], cs_c[:], ALU.mult))
                                S.v(lambda e: e.tensor_tensor(t2[:], psB[:, 0:TT], cs_s[:], ALU.mult))
                                S.v(lambda e: e.tensor_tensor(fm1[:], t1[:], t2[:], ALU.subtract))
                                S.v(lambda e: e.tensor_tensor(t1[:], psA[:, 0:TT], cs_s[:], ALU.mult))
                                S.v(lambda e: e.tensor_tensor(t2[:], psB[:, 0:TT], cs_c[:], ALU.mult))
                                S.v(lambda e: e.tensor_tensor(fm2[:], t1[:], t2[:], ALU.add))
                            elif g0 == O_GQ:
                                S.a(lambda e: e.mul(fm1[:], psA[:, 0:TT], float(128 ** -0.5)))
                                S.a(lambda e: e.mul(fm2[:], psB[:, 0:TT], float(128 ** -0.5)))
                            else:
                                S.v(lambda e: e.tensor_copy(fm1[:], psA[:, 0:TT]))
                                S.v(lambda e: e.tensor_copy(fm2[:], psB[:, 0:TT]))
                            S.dma(qkt[pair[0], :, t0:t0 + TT], fm1[:])
                            S.dma(qkt[pair[1], :, t0:t0 + TT], fm2[:])
                    for bi, g0 in enumerate(TM_COLS):
                        S.dma(wgrp[:], winv[:, :, g0:g0 + 512])
                        for c0 in range(0, TT, C):
                            for kc in range(KC):
                                S.mm(psA[:], xnT[:, kc, c0:c0 + C], wgrp[:, kc, :], kc == 0, kc == KC - 1,
                                     first=(kc == 0), last=(kc == KC - 1))
                            if TM_GATE[bi]:
                                S.a(lambda e: e.activation(tmo[:], psA[:], AF.Silu))
                            else:
                                S.v(lambda e: e.tensor_copy(tmo[:], psA[:]))
                            S.dma(tms[t0 + c0:t0 + c0 + C, bi * 512:(bi + 1) * 512], tmo[:])
                    S.dma(wgrp[:, :, 0:32], winv[:, :, O_GA:O_GA + 32])
                    for kc in range(KC):
                        S.mm(psA[0:32, 0:TT], wgrp[:, kc, 0:32], xnT[:, kc, :], kc == 0, kc == KC - 1,
                             first=(kc == 0), last=(kc == KC - 1))
                    S.v(lambda e: e.tensor_copy(gao[:], psA[0:32, 0:TT]))
                    S.dma(gat[:, t0:t0 + TT], gao[:])

            with contextlib.ExitStack() as p2:
                def sb2(name, shape, dtype):
                    return p2.enter_context(nc.sbuf_tensor(name, shape, dtype))
                woutb = sb2("woutb", [128, KC, D], BF16)
                qk_s = sb2("qk_s", [128, 24, 128], BF16)
                vg_s = sb2("vg_s", [128, 4096], BF16)
                ga_s = sb2("ga_s", [33, 128], F32)
                ez = sb2("ez", [128, 512], F32)
                lz = sb2("lz", [128, 512], F32)
                gtab = sb2("gtab", [128, 3, 128], F32)
                glast = sb2("glast", [128, 1], F32)
                gel = sb2("gel", [128, 1], F32)
                S32 = sb2("S32", [128, 12, 256], F32)
                Sbf = sb2("Sbf", [128, 12, 256], BF16)
                qs = sb2("qs", [128, 2, 128], BF16)
                ks = sb2("ks", [128, 2, 128], BF16)
                kd = sb2("kd", [128, 2, 128], BF16)
                kdt = sb2("kdt", [128, 256], BF16)
                Pm = sb2("Pm", [128, 128], BF16)
                o_s = sb2("o_s", [128, D], F32)
                of_s = sb2("of_s", [128, D], F32)
                ss8 = sb2("ss8", [128, 8], F32)
                mixb = sb2("mixb", [128, D], BF16)
                mixT = sb2("mixT", [128, KC, 128], BF16)
                xh = sb2("xh", [128, D], F32)
                xn2T = sb2("xn2T", [128, KC, 128], F32)
                xn2b = sb2("xn2b", [128, D], BF16)
                lgt = sb2("lgt", [128, NE], F32)
                mx = sb2("mx", [128, 1], F32)
                aft = sb2("aft", [NE, 128], F32)

                for kc in range(KC):
                    S.dma(woutb[:, kc, :], w_out[kc * 128:(kc + 1) * 128, :], eng="gpsimd")
                S.v(lambda e: e.memset(ga_s[:], 1.0))

                seqs = [(0, Ls), (Ls, Lp)]
                for dr in range(2):
                    MASK = MF if dr == 0 else MB
                    U = UF if dr == 0 else UB
                    for (s0, L) in seqs:
                        S.v(lambda e: e.memset(S32[:], 0.0))
                        S.v(lambda e: e.memset(Sbf[:], 0.0))
                        nchs = L // C
                        order = range(nchs) if dr == 0 else range(nchs - 1, -1, -1)
                        for ci in order:
                            r0 = s0 + ci * C
                            S.dma(qk_s[:], qkt[:, :, r0:r0 + C].rearrange("b p t -> p b t"))
                            S.dma(vg_s[:], tms[r0:r0 + C, :])
                            S.dma(ga_s[0:32, :], gat[:, r0:r0 + C])
                            if dr == 1:
                                S.dma(of_s[:], ofw[r0:r0 + C, :])
                            S.mm(psZ[:], ga_s[:], upb_s[:, dr * 512:(dr + 1) * 512], True, True, True, True)
                            S.a(lambda e: e.activation(ez[:], psZ[:], AF.Exp, scale=-1.0))
                            S.a(lambda e: e.activation(lz[:], ez[:], AF.Ln, bias=1.0))
                            for u, H in enumerate(HEADS):
                                nd = H["nd"]
                                h = H["h"]
                                if H["ret"]:
                                    i = dr * 4 + h
                                    Eq = rtab[:, i, 0, :]
                                    Ek = rtab[:, i, 1, :]
                                    Ekd = rtab[:, i, 2, :]
                                    El = rlast[:, i:i + 1]
                                    sl0 = 2 * h
                                else:
                                    S.mm(psS[:], lz[:, 128 * h:128 * (h + 1)], U, True, True, True, True)
                                    lc = 127 if dr == 0 else 0
                                    S.v(lambda e: e.tensor_copy(glast[:], psS[:, lc:lc + 1]))
                                    S.a(lambda e: e.activation(gtab[:, 0, :], psS[:], AF.Exp))
                                    S.a(lambda e: e.activation(gtab[:, 1, :], psS[:], AF.Exp, scale=-1.0))
                                    S.a(lambda e: e.activation(gtab[:, 2, :], psS[:], AF.Exp, scale=-1.0, bias=glast[:, 0:1]))
                                    S.a(lambda e: e.activation(gel[:], glast[:], AF.Exp))
                                    Eq = gtab[:, 0, :]
                                    Ek = gtab[:, 1, :]
                                    Ekd = gtab[:, 2, :]
                                    El = gel[:, 0:1]
                                    sl0 = 8 + h
                                for dc in range(nd):
                                    qb = H["qb"][dc]
                                    kb = H["kb"][dc]
                                    S.v(lambda e: e.tensor_tensor(qs[:, dc, :], qk_s[:, qb, :], Eq, ALU.mult))
                                    S.v(lambda e: e.tensor_tensor(ks[:, dc, :], qk_s[:, kb, :], Ek, ALU.mult))
                                    S.v(lambda e: e.tensor_tensor(kd[:, dc, :], qk_s[:, kb, :], Ekd, ALU.mult))
                                for dc in range(nd):
                                    S.tr(psT[:, dc * 128:(dc + 1) * 128], kd[:, dc, :], identb_s[:],
                                         first=(dc == 0), last=(dc == nd - 1))
                                S.v(lambda e: e.tensor_copy(kdt[:, 0:nd * 128], psT[:, 0:nd * 128]))
                                for dc in range(nd):
                                    S.mm(psS[:], ks[:, dc, :], qs[:, dc, :], dc == 0, dc == nd - 1,
                                         first=(dc == 0), last=(dc == nd - 1))
                                S.v(lambda e: e.tensor_tensor(Pm[:], psS[:], MASK, ALU.mult))
                                vv = vg_s[:, H["v"]:H["v"] + 256]
                                S.mm(psO[:], Pm[:], vv, True, False, True, False)
                                for dc in range(nd):
                                    S.mm(psO[:], qs[:, dc, :], Sbf[:, sl0 + dc, :], False, dc == nd - 1,
                                         first=False, last=(dc == nd - 1))
                                osl = o_s[:, 256 * u:256 * (u + 1)]
                                if dr == 0:
                                    S.v(lambda e: e.tensor_copy(osl, psO[:]))
                                else:
                                    S.v(lambda e: e.tensor_tensor(osl, psO[:], of_s[:, 256 * u:256 * (u + 1)], ALU.add))
                                for dc in range(nd):
                                    S.mm(psSt[:, dc, :], kdt[:, dc * 128:(dc + 1) * 128], vv, True, True,
                                         first=(dc == 0), last=(dc == nd - 1))
                                for dc in range(nd):
                                    S.v(lambda e: e.scalar_tensor_tensor(S32[:, sl0 + dc, :], S32[:, sl0 + dc, :], El,
                                                                         psSt[:, dc, :], ALU.mult, ALU.add))
                                    S.a(lambda e: e.copy(Sbf[:, sl0 + dc, :], S32[:, sl0 + dc, :]))
                            if dr == 0:
                                S.dma(ofw[r0:r0 + C, :], o_s[:])
                                continue
                            S.v(lambda e: e.tensor_tensor(of_s[:], o_s[:], o_s[:], ALU.mult))
                            S.v(lambda e: e.reduce_sum(ss8[:], of_s[:].rearrange("p (u v) -> p u v", v=256), AX.X))
                            S.v(lambda e: e.tensor_scalar(ss8[:], ss8[:], 1.0 / 256.0, EPS, ALU.mult, ALU.add))
                            S.a(lambda e: e.activation(ss8[:], ss8[:], AF.Sqrt))
                            S.v(lambda e: e.reciprocal(ss8[:], ss8[:]))
                            for u, H in enumerate(HEADS):
                                S.v(lambda e: e.scalar_tensor_tensor(mixb[:, 256 * u:256 * (u + 1)], o_s[:, 256 * u:256 * (u + 1)],
                                                                     ss8[:, u:u + 1], vg_s[:, H["g"]:H["g"] + 256],
                                                                     ALU.mult, ALU.mult))
                            for k0 in range(0, KC, 8):
                                for kk in range(8):
                                    kc = k0 + kk
                                    S.tr(psT[:, kk * 128:(kk + 1) * 128], mixb[:, kc * 128:(kc + 1) * 128], identb_s[:],
                                         first=(kk == 0), last=(kk == 7))
                                for kk in range(8):
                                    kc = k0 + kk
                                    S.v(lambda e: e.tensor_scalar(mixT[:, kc, :], psT[:, kk * 128:(kk + 1) * 128],
                                                                  gnw_s[:, kc:kc + 1], None, ALU.mult))
                            S.dma(xh[:], x_in[r0:r0 + C, :])
                            for cb in range(4):
                                for kc in range(KC):
                                    S.mm(psA[:], mixT[:, kc, :], woutb[:, kc, cb * 512:(cb + 1) * 512], kc == 0, kc == KC - 1,
                                         first=(kc == 0), last=(kc == KC - 1))
                                S.v(lambda e: e.tensor_tensor(xh[:, cb * 512:(cb + 1) * 512], xh[:, cb * 512:(cb + 1) * 512],
                                                              psA[:], ALU.add))
                            S.dma(hbuf[r0:r0 + C, :], xh[:])
                            if debug:
                                S.dma(dbg_h[r0:r0 + C, :], xh[:])
                            S.v(lambda e: e.tensor_tensor(of_s[:], xh[:], xh[:], ALU.mult))
                            S.v(lambda e: e.reduce_sum(mx[:], of_s[:], AX.X))
                            S.v(lambda e: e.tensor_scalar(mx[:], mx[:], 1.0 / D, EPS, ALU.mult, ALU.add))
                            S.a(lambda e: e.activation(mx[:], mx[:], AF.Sqrt))
                            S.v(lambda e: e.reciprocal(mx[:], mx[:]))
                            S.v(lambda e: e.tensor_scalar(of_s[:], xh[:], mx[:, 0:1], None, ALU.mult))
                            S.v(lambda e: e.tensor_copy(xn2b[:], of_s[:]))
                            if s0 == 0:
                                S.dma(xn_l[r0:r0 + C, :], xn2b[:])
                            else:
                                S.dma(xn_p[r0 - Ls:r0 - Ls + C, :], xn2b[:])
                            for k0 in range(0, KC, 4):
                                for kk in range(4):
                                    kc = k0 + kk
                                    S.tr(psTf[:, kk * 128:(kk + 1) * 128], of_s[:, kc * 128:(kc + 1) * 128], identf_s[:],
                                         first=(kk == 0), last=(kk == 3))
                                for kk in range(4):
                                    kc = k0 + kk
                                    S.v(lambda e: e.tensor_scalar(xn2T[:, kc, :], psTf[:, kk * 128:(kk + 1) * 128],
                                                                  n2w_s[:, kc:kc + 1], None, ALU.mult))
                            for kc in range(KC):
                                S.mm(psS[:, 0:NE], xn2T[:, kc, :], rw_s[:, kc * NE:(kc + 1) * NE], kc == 0, kc == KC - 1,
                                     first=(kc == 0), last=(kc == KC - 1))
                            S.v(lambda e: e.reduce_max(mx[:], psS[:, 0:NE], AX.X))
                            S.v(lambda e: e.tensor_scalar(mx[:], mx[:], -1.0, None, ALU.mult))
                            S.a(lambda e: e.activation(lgt[:], psS[:, 0:NE], AF.Exp, bias=mx[:, 0:1]))
                            S.v(lambda e: e.reduce_sum(mx[:], lgt[:], AX.X))
                            S.v(lambda e: e.reciprocal(mx[:], mx[:]))
                            S.v(lambda e: e.tensor_scalar(lgt[:], lgt[:], mx[:, 0:1], None, ALU.mult))
                            S.tr(psTf[0:NE, 0:128], lgt[:, 0:NE], identf_s[:])
                            S.v(lambda e: e.tensor_copy(aft[:], psTf[0:NE, 0:128]))
                            if s0 == 0:
                                S.dma(affb[:, ci * C:(ci + 1) * C], aft[:])
                            else:
                                S.dma(affx_p.rearrange("(e p) j -> e (p j)", p=128)[:, ci * C:(ci + 1) * C], aft[:])
        if phase == 2:
            for (src, dst) in ((wg, wgb), (wu, wub), (wd, wdb)):
                for k in range(2):
                    for r0 in range(0, D, RB):
                        S.dma(dst[k, r0:r0 + RB, :], src[k, r0:r0 + RB, :], eng="gpsimd")
            with contextlib.ExitStack() as pz:
                ztf = pz.enter_context(nc.sbuf_tensor("ztf", [128, 8192], F32))
                S.v(lambda e: e.memset(ztf[:], 0.0))

                def zero_fill(buf, n, zt, per):
                    v = buf.rearrange("r c -> (r c)")
                    for o in range(0, n, per):
                        m = min(per, n - o)
                        S.dma(v[o:o + m].rearrange("(p f) -> p f", p=128), zt[:, 0:m // 128])
                zero_fill(out_s, TS * D, ztf, 128 * 8192)
                zero_fill(out_p, Lp * D, ztf, 128 * 8192)

            JM = max(JS, JP)
            with contextlib.ExitStack() as p4:
                def sb4(name, shape, dtype):
                    return p4.enter_context(nc.sbuf_tensor(name, shape, dtype))
                auxs = sb4("auxs", [128, 258], F32)
                onesm = sb4("onesm", [128, 256], F32)
                eidx_s = sb4("eidx_s", [128, 2], I32)
                A = sb4("A", [128, JM], F32)
                Mk = sb4("Mk", [128, JM], F32)
                cntl = sb4("cntl", [128, JM], F32)
                junk = sb4("junk", [128, JM], F32)
                lo = sb4("lo", [128, 1], F32)
                hi = sb4("hi", [128, 1], F32)
                mid = sb4("mid", [128, 1], F32)
                cntp = sb4("cntp", [128, 1], F32)
                gem = sb4("gem", [128, 1], U32)
                ltm = sb4("ltm", [128, 1], U32)
                rinc = sb4("rinc", [128, 1], F32)
                rexc = sb4("rexc", [128, 1], F32)
                acol = sb4("acol", [128, 1], F32)
                bcol = sb4("bcol", [128, 1], F32)
                xcol = sb4("xcol", [128, 2], F32)
                selx = sb4("selx", [128, 2], F32)
                sloc = sb4("sloc", [128, 1], F32)
                jcol = sb4("jcol", [128, 1], F32)
                tf = sb4("tf", [128, 1], F32)
                G2 = sb4("G2", [128, 128], F32)
                OH = sb4("OH", [128, 128], F32)
                NBM = max(capS, capP) // 128
                idx_all = sb4("idx_all", [128, NBM], I32)
                gate_all = sb4("gate_all", [128, NBM], F32)
                xg = sb4("xg", [128, D], BF16)
                XT = sb4("XT", [128, KC, 512], BF16)
                hT = sb4("hT", [128, KC, 512], BF16)
                wA = sb4("wA", [128, KC, 512], BF16)
                wB = sb4("wB", [128, KC, 512], BF16)
                sg = sb4("sg", [128, 512], F32)
                ye = sb4("ye", [128, 4, D], F32)
                S.dma(auxs[:], aux)
                S.dma(eidx_s[:], eidx)
                S.v(lambda e: e.memset(onesm[:], 1.0))
                for k in range(2):
                    for (J, cap, XN, AFFX, OUT) in ((JS, capS, xn_s, affx_s, out_s), (JP, capP, xn_p, affx_p, out_p)):
                        NB = cap // 128
                        S.op("gpsimd", lambda e: e.indirect_dma_start(
                            out=A[:, 0:J], out_offset=None, in_=AFFX[:, :],
                            in_offset=bass.IndirectOffsetOnAxis(ap=eidx_s[:, k:k + 1], axis=0)), dma=True)
                        S.v(lambda e: e.memset(lo[:], 0.0))
                        S.v(lambda e: e.memset(hi[:], 2.0))
                        for it in range(36):
                            S.v(lambda e: e.tensor_tensor(mid[:], lo[:], hi[:], ALU.add))
                            S.v(lambda e: e.tensor_scalar(mid[:], mid[:], 0.5, None, ALU.mult))
                            S.v(lambda e: e.tensor_scalar(junk[:, 0:J], A[:, 0:J], mid[:, 0:1], None, ALU.is_ge))
                            S.v(lambda e: e.reduce_sum(cntp[:], junk[:, 0:J], AX.X))
                            S.mm(psO[:, 0:1], onesm[:, 0:128], cntp[:], True, True, True, True)
                            S.v(lambda e: e.tensor_scalar(gem[:], psO[:, 0:1], float(cap), None, ALU.is_ge))
                            S.v(lambda e: e.tensor_scalar(ltm[:], psO[:, 0:1], float(cap), None, ALU.is_lt))
                            S.v(lambda e: e.copy_predicated(lo[:], gem[:], mid[:]))
                            S.v(lambda e: e.copy_predicated(hi[:], ltm[:], mid[:]))
                        S.v(lambda e: e.tensor_scalar(Mk[:, 0:J], A[:, 0:J], lo[:, 0:1], None, ALU.is_ge))
                        S.v(lambda e: e.tensor_tensor_scan(cntl[:, 0:J], onesm[:, 0:J], Mk[:, 0:J], 0.0, ALU.mult, ALU.add))
                        S.mm(psO[:, 1:2], MF, cntl[:, J - 1:J], True, True, True, True)
                        S.v(lambda e: e.tensor_copy(rinc[:], psO[:, 1:2]))
                        S.v(lambda e: e.tensor_tensor(rexc[:], rinc[:], cntl[:, J - 1:J], ALU.subtract))
                        S.v(lambda e: e.tensor_copy(xcol[:, 0:1], rexc[:]))
                        S.v(lambda e: e.tensor_copy(xcol[:, 1:2], auxs[:, 256:257]))
                        for b0 in range(0, NB, 4):
                            nbt = min(4, NB - b0)
                            N = nbt * 128
                            for bi in range(nbt):
                                b = b0 + bi
                                sbase = 128 * b
                                S.v(lambda e: e.tensor_scalar(acol[:], rexc[:], float(1 - sbase), None, ALU.add))
                                S.v(lambda e: e.tensor_scalar(bcol[:], rinc[:], float(1 - sbase), None, ALU.add))
                                S.v(lambda e: e.tensor_scalar(G2[:], POSF, acol[:, 0:1], None, ALU.is_ge))
                                S.v(lambda e: e.scalar_tensor_tensor(OH[:], POSF, bcol[:, 0:1], G2[:], ALU.is_lt, ALU.mult))
                                S.mm(psZ[:, 0:J], OH[:], cntl[:, 0:J], True, True, True, False)
                                S.mm(psTf[:, 0:J], OH[:], A[:, 0:J], True, True, False, False)
                                S.mm(psS[:, 0:2], OH[:], xcol[:], True, True, False, True)
                                S.v(lambda e: e.tensor_copy(selx[:], psS[:, 0:2]))
                                S.v(lambda e: e.scalar_tensor_tensor(sloc[:], auxs[:, 256:257], float(sbase), selx[:, 0:1],
                                                                     ALU.add, ALU.subtract))
                                S.v(lambda e: e.tensor_scalar(junk[:, 0:J], psZ[:, 0:J], sloc[:, 0:1], None, ALU.is_le))
                                S.v(lambda e: e.reduce_sum(jcol[:], junk[:, 0:J], AX.X))
                                S.v(lambda e: e.scalar_tensor_tensor(tf[:], selx[:, 1:2], float(J), jcol[:], ALU.mult, ALU.add))
                                S.v(lambda e: e.tensor_copy(idx_all[:, b:b + 1], tf[:]))
                                S.v(lambda e: e.tensor_scalar(junk[:, 0:J], auxs[:, 0:J], jcol[:, 0:1], None, ALU.is_equal))
                                S.v(lambda e: e.tensor_tensor(junk[:, 0:J], junk[:, 0:J], psTf[:, 0:J], ALU.mult))
                                S.v(lambda e: e.reduce_sum(gate_all[:, b:b + 1], junk[:, 0:J], AX.X))
                                S.op("gpsimd", lambda e: e.indirect_dma_start(
                                    out=xg[:, :], out_offset=None, in_=XN[:, :],
                                    in_offset=bass.IndirectOffsetOnAxis(ap=idx_all[:, b:b + 1], axis=0)), dma=True)
                                for k0 in range(0, KC, 8):
                                    for kk in range(8):
                                        kc = k0 + kk
                                        S.tr(psT[:, kk * 128:(kk + 1) * 128], xg[:, kc * 128:(kc + 1) * 128], identb_s[:],
                                             first=(kk == 0), last=(kk == 7))
                                    for kk in range(8):
                                        kc = k0 + kk
                                        S.v(lambda e: e.tensor_scalar(XT[:, kc, bi * 128:(bi + 1) * 128],
                                                                      psT[:, kk * 128:(kk + 1) * 128],
                                                                      n2w_s[:, kc:kc + 1], None, ALU.mult))
                            wgv = wgb[k].rearrange("(kc p) f -> p kc f", p=128)
                            wuv = wub[k].rearrange("(kc p) f -> p kc f", p=128)
                            wdv = wdb[k].rearrange("(kc p) f -> p kc f", p=128)
                            for fg in range(4):
                                S.dma(wA[:], wgv[:, :, fg * 512:(fg + 1) * 512])
                                S.dma(wB[:], wuv[:, :, fg * 512:(fg + 1) * 512])
                                for fi in range(4):
                                    fc = fg * 4 + fi
                                    for kc in range(KC):
                                        S.mm(psA[:, 0:N], wA[:, kc, fi * 128:(fi + 1) * 128], XT[:, kc, 0:N], kc == 0, kc == KC - 1,
                                             first=(kc == 0), last=False)
                                    for kc in range(KC):
                                        S.mm(psB[:, 0:N], wB[:, kc, fi * 128:(fi + 1) * 128], XT[:, kc, 0:N], kc == 0, kc == KC - 1,
                                             first=False, last=(kc == KC - 1))
                                    S.a(lambda e: e.activation(sg[:, 0:N], psA[:, 0:N], AF.Silu))
                                    S.v(lambda e: e.tensor_tensor(hT[:, fc, 0:N], sg[:, 0:N], psB[:, 0:N], ALU.mult))
                            for cb in range(4):
                                S.dma(wA[:], wdv[:, :, cb * 512:(cb + 1) * 512])
                                for bi in range(nbt):
                                    for fc in range(KC):
                                        S.mm(psA[:], hT[:, fc, bi * 128:(bi + 1) * 128], wA[:, fc, :], fc == 0, fc == KC - 1,
                                             first=(fc == 0), last=(fc == KC - 1))
                                    S.v(lambda e: e.tensor_scalar(ye[:, bi, cb * 512:(cb + 1) * 512], psA[:],
                                                                  gate_all[:, b0 + bi:b0 + bi + 1], None, ALU.mult))
                            for bi in range(nbt):
                                S.op("gpsimd", lambda e: e.indirect_dma_start(
                                    out=OUT[:, :], out_offset=bass.IndirectOffsetOnAxis(ap=idx_all[:, b0 + bi:b0 + bi + 1], axis=0),
                                    in_=ye[:, bi, :], in_offset=None, compute_op=ALU.add), dma=True)

        if phase == 3:
            with contextlib.ExitStack() as p5:
                def sb5(name, shape, dtype):
                    return p5.enter_context(nc.sbuf_tensor(name, shape, dtype))
                nfr = sb5("nfr", [128, D], F32)
                hh = sb5("hh", [128, D], F32)
                og = sb5("og", [128, NCORES, D], F32)
                r1 = sb5("r1", [128, 1], F32)
                S.dma(nfr[:], nfw.to_broadcast([128, D]))
                for ci in range(NR // C):
                    r0 = ci * C
                    S.dma(hh[:], h_own[r0:r0 + C, :])
                    S.dma(og[:], contrib[:, r0:r0 + C, :].rearrange("k p d -> p k d"))
                    for kk in range(NCORES):
                        S.v(lambda e: e.tensor_tensor(hh[:], hh[:], og[:, kk, :], ALU.add))
                    S.v(lambda e: e.tensor_tensor(og[:, 0, :], hh[:], hh[:], ALU.mult))
                    S.v(lambda e: e.reduce_sum(r1[:], og[:, 0, :], AX.X))
                    S.v(lambda e: e.tensor_scalar(r1[:], r1[:], 1.0 / D, EPS, ALU.mult, ALU.add))
                    S.a(lambda e: e.activation(r1[:], r1[:], AF.Sqrt))
                    S.v(lambda e: e.reciprocal(r1[:], r1[:]))
                    S.v(lambda e: e.tensor_scalar(hh[:], hh[:], r1[:, 0:1], None, ALU.mult))
                    S.v(lambda e: e.tensor_tensor(hh[:], hh[:], nfr[:], ALU.mult))
                    S.dma(y_o[r0:r0 + C, :], hh[:])
        nc.sync.wait_ge(S.last[0], S.last[1])
    return nc


def _consts(Ls, Lp):
    NT = Ls + Lp
    inv = (10000.0 ** (-np.arange(0, 256, 2, dtype=np.float32) / np.float32(256))).astype(np.float32)
    pos = np.concatenate([np.arange(Ls, dtype=np.float32), np.arange(Lp, dtype=np.float32)])
    ang = (pos[None, :] * inv[:, None]).astype(np.float32)
    ropec = np.cos(ang).astype(np.float32)
    ropes = np.sin(ang).astype(np.float32)
    i = np.arange(128, dtype=np.float32)
    one = np.ones((128, 1), np.float32)
    POSF = one * (i + 1)[None, :]
    POSB = one * (128 - i)[None, :]
    CF = 128 - POSF
    CB = 128 - POSB
    jj = np.arange(128)[:, None]
    ii = np.arange(128)[None, :]
    MF = (ii >= jj).astype(np.float32)
    MB = (ii <= jj).astype(np.float32)
    UF = MF * np.float32(-1.0 / 16.0)
    UB = MB * np.float32(-1.0 / 16.0)
    ctab = np.concatenate([POSF, POSB, CF, CB, MF, MB, UF, UB], axis=1).astype(np.float32)
    return ropec, ropes, ctab


def _affidx(c):
    r = np.arange(256)
    e, q = r // 16, r % 16
    return np.ascontiguousarray((e * 128 + 16 * c + q).astype(np.int32).reshape(2, 128).T)


def _eidx(c):
    p = np.arange(128)
    return np.ascontiguousarray(np.stack([(2 * c) * 128 + p, (2 * c + 1) * 128 + p], axis=1).astype(np.int32))


def _aux():
    a = np.zeros((128, 258), np.float32)
    a[:, 0:256] = np.arange(256, dtype=np.float32)[None, :]
    a[:, 256] = np.arange(128, dtype=np.float32)
    return a


def _colmajor(w):
    return np.ascontiguousarray(np.asarray(w, np.float32).reshape(KC, 128).T)


def _run(nc, maps):
    res = run_bass_kernel_spmd(nc, maps, core_ids=list(range(NCORES)))
    return res.results


def kernel(**inputs):
    f32 = np.float32
    x_prompt, x_sample = inputs["x_prompt"], inputs["x_sample"]
    Ls = x_sample.shape[1]
    Lp = x_prompt.shape[1]
    TS = NCORES * Ls
    JS = TS // 128
    LQ = Lp // NCORES
    ropec, ropes, ctab = _consts(Ls, Lp)
    identf = np.eye(128, dtype=f32)
    n2w = _colmajor(inputs["norm2_w"][0])
    upb = np.zeros((33, 1024), f32)
    upb[0:16, 0:512] = inputs["gla_gate_up"][0, 0]
    upb[16:32, 512:1024] = inputs["gla_gate_up"][0, 1]
    upb[32, 0:512] = inputs["gla_gate_bias"][0, 0]
    upb[32, 512:1024] = inputs["gla_gate_bias"][0, 1]
    rwl = np.ascontiguousarray(np.asarray(inputs["router_w"][0], f32).reshape(KC, 128, NE).transpose(1, 0, 2).reshape(128, KC * NE))
    xp = np.asarray(x_prompt[0], f32)
    common1 = dict(
        w_in=np.ascontiguousarray(inputs["w_in"][0], dtype=f32), w_out=np.ascontiguousarray(inputs["w_out"][0], dtype=f32),
        n1w=_colmajor(inputs["norm1_w"][0]), n2w=n2w,
        gnw=_colmajor(np.concatenate([inputs["ret_gn_w"][0], inputs["gla_gn_w"][0]])),
        declog=np.ascontiguousarray(np.asarray(inputs["ret_decay_logit"][0], f32).reshape(1, 8)),
        upb=upb, rw=rwl, ropec=ropec, ropes=ropes, ctab=ctab, identf=identf)
    maps1 = [dict(common1, x_in=np.ascontiguousarray(np.concatenate([np.asarray(x_sample[c], f32), xp], axis=0)))
             for c in range(NCORES)]
    r1 = _run(build(Ls, Lp, 1), maps1)
    del maps1
    xn_s = np.ascontiguousarray(np.concatenate([r1[c]["xn_l"] for c in range(NCORES)], axis=0))
    affb = np.stack([np.asarray(r1[c]["affb"], f32) for c in range(NCORES)], axis=0)
    affx_s = np.ascontiguousarray(affb.reshape(NCORES, NE, 16, JS).transpose(1, 0, 2, 3).reshape(NE * 128, JS))
    xn_p = np.ascontiguousarray(r1[0]["xn_p"])
    affx_p = np.ascontiguousarray(r1[0]["affx_p"])
    h_own = [np.ascontiguousarray(np.concatenate([r1[c]["hbuf"][:Ls], r1[c]["hbuf"][Ls + c * LQ:Ls + (c + 1) * LQ]], axis=0))
             for c in range(NCORES)]
    del r1
    maps2 = [dict(wg=np.ascontiguousarray(inputs["w_gate"][0, 2 * c:2 * c + 2], dtype=f32),
                  wu=np.ascontiguousarray(inputs["w_up"][0, 2 * c:2 * c + 2], dtype=f32),
                  wd=np.ascontiguousarray(inputs["w_down"][0, 2 * c:2 * c + 2], dtype=f32),
                  eidx=_eidx(c), aux=_aux(), xn_s=xn_s, xn_p=xn_p, affx_s=affx_s, affx_p=affx_p,
                  n2w=n2w, ctab=ctab, identf=identf) for c in range(NCORES)]
    r2 = _run(build(Ls, Lp, 2), maps2)
    del maps2
    maps3 = []
    for c in range(NCORES):
        contrib = np.stack([np.concatenate([r2[k]["out_s"][c * Ls:(c + 1) * Ls], r2[k]["out_p"][c * LQ:(c + 1) * LQ]], axis=0)
                            for k in range(NCORES)], axis=0)
        maps3.append(dict(nfw=np.ascontiguousarray(np.asarray(inputs["normf_w"], f32).reshape(1, D)),
                          h_own=h_own[c], contrib=np.ascontiguousarray(contrib, dtype=f32)))
    del r2
    r3 = _run(build(Ls, Lp, 3), maps3)
    y_s = np.stack([r3[c]["y_o"][:Ls] for c in range(NCORES)], axis=0).astype(f32)
    y_p = np.concatenate([r3[c]["y_o"][Ls:] for c in range(NCORES)], axis=0)[None].astype(f32)
    return (y_p, y_s)
```
